# Optimizing a Trainium2 kernel written in Bass

```python
import jax, jax.numpy as jnp
from jax import lax
import numpy as np

D_MODEL = 1024
BATCH = 8
SEQ = 4096
DEPTH = 4

D_MIX = D_MODEL
D_POOL = D_MIX // 2
N_POOL_GROUPS = 4
POOL_GROUP = D_POOL // N_POOL_GROUPS
POOL_WINDOWS = (2, 4, 8, 16)
N_MLA_HEADS = 4
QK_NOPE = 128
QK_ROPE = 64
V_HEAD = 128
D_MLA = N_MLA_HEADS * V_HEAD
Q_LORA = 256
KV_LORA = 128
D_IN = D_POOL + Q_LORA + KV_LORA + QK_ROPE
ROPE_THETA = 10000.0
Q_BLOCK = 128
N_MEM = 256
N_XHEADS = 4
XHEAD = D_MODEL // N_XHEADS
N_EXPERTS = 16
CAPACITY_FACTOR = 2
D_EXPERT = 2 * D_MODEL
EPS = 1e-6

kernel_name = "hybrid_pool_mla_memxattn_ecmoe_encoder"


def rmsnorm(t, gain):
    t32 = t.astype(jnp.float32)
    y = t32 * lax.rsqrt(jnp.mean(t32 * t32, axis=-1, keepdims=True) + EPS) * gain.astype(jnp.float32)
    return y.astype(t.dtype)


def apply_rope(t, cos, sin):
    t1, t2 = jnp.split(t.astype(jnp.float32), 2, axis=-1)
    return jnp.concatenate([t1 * cos - t2 * sin, t1 * sin + t2 * cos], axis=-1).astype(t.dtype)


def pool_mixer(u, w_pool, pool_scale):
    B, S, _ = u.shape
    u32 = u.astype(jnp.float32)
    cs = jnp.concatenate([jnp.zeros((B, 1, D_POOL), jnp.float32), jnp.cumsum(u32, axis=1)], axis=1)
    t = jnp.arange(S)
    outs = []
    for g, w in enumerate(POOL_WINDOWS):
        left = w // 2
        right = w - 1 - left
        lo = jnp.clip(t - left, 0, S)
        hi = jnp.clip(t + right + 1, 0, S)
        sl = slice(g * POOL_GROUP, (g + 1) * POOL_GROUP)
        csg = cs[:, :, sl]
        mean = (csg[:, hi] - csg[:, lo]) / (hi - lo).astype(jnp.float32)[:, None]
        outs.append(jnp.einsum('bsc,cd->bsd', (mean - u32[:, :, sl]).astype(u.dtype), w_pool[g]))
    return jnp.concatenate(outs, axis=-1) * pool_scale


def mla_attention(q_nope, q_rope, k_nope, k_rope, v):
    B, S, H, _ = q_nope.shape
    nb = S // Q_BLOCK
    scale = (QK_NOPE + QK_ROPE) ** -0.5

    def blocks(t):
        return jnp.moveaxis(t.reshape(B, nb, Q_BLOCK, *t.shape[2:]), 1, 0)

    def one_block(args):
        qn, qr = args
        s = jnp.einsum('bqhd,bkhd->bhqk', qn, k_nope) + jnp.einsum('bqhr,bkr->bhqk', qr, k_rope)
        p = jax.nn.softmax(s.astype(jnp.float32) * scale, axis=-1).astype(v.dtype)
        return jnp.einsum('bhqk,bkhd->bqhd', p, v)

    o = lax.map(one_block, (blocks(q_nope), blocks(q_rope)))
    return jnp.moveaxis(o, 0, 1).reshape(B, S, H * V_HEAD)


def memory_cross_attention(h, m, wq, wkv, wo):
    B, S, _ = h.shape
    M = m.shape[1]
    q = (h @ wq).reshape(B, S, N_XHEADS, XHEAD)
    k, v = jnp.split(m @ wkv, 2, axis=-1)
    k = k.reshape(B, M, N_XHEADS, XHEAD)
    v = v.reshape(B, M, N_XHEADS, XHEAD)
    s = jnp.einsum('bqhd,bkhd->bhqk', q, k).astype(jnp.float32) * (XHEAD ** -0.5)
    p = jax.nn.softmax(s, axis=-1).astype(v.dtype)
    o = jnp.einsum('bhqk,bkhd->bqhd', p, v).reshape(B, S, D_MODEL)
    return o @ wo


def expert_choice_ffn(h, w_router, w_gate, w_up, w_down):
    B, S, D = h.shape
    cap = CAPACITY_FACTOR * S // N_EXPERTS
    affinity = jax.nn.softmax(h.astype(jnp.float32) @ w_router.astype(jnp.float32), axis=-1)
    gates, idx = lax.top_k(jnp.swapaxes(affinity, 1, 2), cap)
    b_idx = jnp.arange(B)[:, None, None]
    xt = h[b_idx, idx]
    a = jnp.einsum('becd,edf->becf', xt, w_gate)
    g = jnp.einsum('becd,edf->becf', xt, w_up)
    y = jnp.einsum('becf,efd->becd', jax.nn.silu(a) * g, w_down) * gates[..., None].astype(h.dtype)
    return jnp.zeros_like(h).at[b_idx, idx].add(y)


def setup_inputs(seed: int = 0) -> dict:
    key = jax.random.key(seed)
    ks = jax.random.split(key, 24)
    L = DEPTH

    def w(k, shape, fan_in):
        return jax.random.normal(k, shape, jnp.float32) * (fan_in ** -0.5)

    def gain(k, shape):
        return 1.0 + 0.05 * jax.random.normal(k, shape, jnp.float32)

    x = jax.random.normal(ks[0], (BATCH, SEQ, D_MODEL), jnp.float32)
    mem = jax.random.normal(ks[1], (BATCH, N_MEM, D_MODEL), jnp.float32)
    offsets = jax.random.randint(ks[2], (BATCH, 1), 0, 1024, dtype=jnp.int32)
    positions = offsets + jnp.arange(SEQ, dtype=jnp.int32)[None, :]
    return {
        "x": x,
        "mem": mem,
        "positions": positions,
        "norm_mix": gain(ks[3], (L, D_MODEL)),
        "w_in": w(ks[4], (L, D_MODEL, D_IN), D_MODEL),
        "q_norm": gain(ks[5], (L, Q_LORA)),
        "kv_norm": gain(ks[6], (L, KV_LORA)),
        "w_uq": w(ks[7], (L, Q_LORA, N_MLA_HEADS * (QK_NOPE + QK_ROPE)), Q_LORA),
        "w_ukv": w(ks[8], (L, KV_LORA, N_MLA_HEADS * (QK_NOPE + V_HEAD)), KV_LORA),
        "w_pool": w(ks[9], (L, N_POOL_GROUPS, POOL_GROUP, POOL_GROUP), POOL_GROUP),
        "pool_scale": gain(ks[10], (L, D_POOL)),
        "w_out": w(ks[11], (L, D_MIX, D_MODEL), D_MIX),
        "norm_x": gain(ks[12], (L, D_MODEL)),
        "mem_norm": gain(ks[13], (L, D_MODEL)),
        "wx_q": w(ks[14], (L, D_MODEL, D_MODEL), D_MODEL),
        "wx_kv": w(ks[15], (L, D_MODEL, 2 * D_MODEL), D_MODEL),
        "wx_o": w(ks[16], (L, D_MODEL, D_MODEL), D_MODEL),
        "norm_ffn": gain(ks[17], (L, D_MODEL)),
        "w_router": w(ks[18], (L, D_MODEL, N_EXPERTS), D_MODEL),
        "w_gate": w(ks[19], (L, N_EXPERTS, D_MODEL, D_EXPERT), D_MODEL),
        "w_up": w(ks[20], (L, N_EXPERTS, D_MODEL, D_EXPERT), D_MODEL),
        "w_down": w(ks[21], (L, N_EXPERTS, D_EXPERT, D_MODEL), D_EXPERT),
        "norm_final": gain(ks[22], (D_MODEL,)),
    }


def reference(x, mem, positions, norm_mix, w_in, q_norm, kv_norm, w_uq, w_ukv, w_pool, pool_scale,
              w_out, norm_x, mem_norm, wx_q, wx_kv, wx_o, norm_ffn, w_router, w_gate, w_up, w_down,
              norm_final):
    B, S, _ = x.shape
    half = QK_ROPE // 2
    inv_freq = ROPE_THETA ** (-jnp.arange(half, dtype=jnp.float32) / half)
    ang = positions.astype(jnp.float32)[..., None] * inv_freq
    cos, sin = jnp.cos(ang), jnp.sin(ang)
    splits = [D_POOL, D_POOL + Q_LORA, D_POOL + Q_LORA + KV_LORA]

    for l in range(DEPTH):
        h = rmsnorm(x, norm_mix[l])
        proj = h @ w_in[l]
        u, c_q, c_kv, k_r = jnp.split(proj, splits, axis=-1)
        pool_out = pool_mixer(u, w_pool[l], pool_scale[l])
        q = (rmsnorm(c_q, q_norm[l]) @ w_uq[l]).reshape(B, S, N_MLA_HEADS, QK_NOPE + QK_ROPE)
        q_nope, q_rope = jnp.split(q, [QK_NOPE], axis=-1)
        kv = (rmsnorm(c_kv, kv_norm[l]) @ w_ukv[l]).reshape(B, S, N_MLA_HEADS, QK_NOPE + V_HEAD)
        k_nope, v = jnp.split(kv, [QK_NOPE], axis=-1)
        q_rope = apply_rope(q_rope, cos[:, :, None, :], sin[:, :, None, :])
        k_rope = apply_rope(k_r, cos, sin)
        mla_out = mla_attention(q_nope, q_rope, k_nope, k_rope, v)
        x = x + jnp.concatenate([pool_out, mla_out], axis=-1) @ w_out[l]

        h = rmsnorm(x, norm_x[l])
        m = rmsnorm(mem, mem_norm[l])
        x = x + memory_cross_attention(h, m, wx_q[l], wx_kv[l], wx_o[l])

        h = rmsnorm(x, norm_ffn[l])
        x = x + expert_choice_ffn(h, w_router[l], w_gate[l], w_up[l], w_down[l])

    return rmsnorm(x, norm_final)
```

```python
import contextlib
import numpy as np
import concourse.bass as bass
import concourse.mybir as mybir
from concourse.bass_utils import run_bass_kernel_spmd
from concourse.alu_op_type import AluOpType as ALU

F32 = mybir.dt.float32
BF16 = mybir.dt.bfloat16
I32 = mybir.dt.int32
AF = mybir.ActivationFunctionType

SEQ = 4096
DM = 1024
NT = SEQ // 512
EPS = 1e-6
NEXP = 16
CAP = 512
DEPTH = 4


class Res:
    __slots__ = ("w", "r")

    def __init__(self):
        self.w = None
        self.r = {}


class _Eng:
    def __init__(self, name, sem):
        self.name = name
        self.sem = sem
        self.count = 0
        self.seen = {}
        self.ops = []
        self.dma_i = 0
        self.dma_sems = []


class Sched:
    ENGS = ("tensor", "vector", "scalar", "gpsimd", "sync")

    def __init__(self, nc, stack, n_dma_sems=16):
        self.e = {}
        for n in self.ENGS:
            self.e[n] = _Eng(n, stack.enter_context(nc.semaphore("sem_" + n)))
        for n in ("sync", "gpsimd"):
            self.e[n].dma_sems = [stack.enter_context(nc.semaphore(f"dsem_{n}_{i}"))
                                  for i in range(n_dma_sems)]
        self.n_ops = 0
        self.cc_sem = stack.enter_context(nc.semaphore("cc_sem"))
        self.cc_count = 0

    def _deps(self, eng, reads, writes, extra=()):
        deps = {}

        def add(t):
            if t is None:
                return
            k = id(t[0])
            if k not in deps or deps[k][1] < t[1]:
                deps[k] = t
        for r in reads:
            add(r.w)
        for w in writes:
            add(w.w)
            for t in w.r.values():
                add(t)
        for t in extra:
            add(t)
        waits = []
        for k, t in deps.items():
            if t[0] is eng.sem and t[1] > eng.count:
                continue
            if eng.seen.get(k, 0) >= t[1]:
                continue
            eng.seen[k] = t[1]
            waits.append(t)
        return waits

    @staticmethod
    def _commit(tok, reads, writes):
        k = id(tok[0])
        for r in reads:
            if k not in r.r or r.r[k][1] < tok[1]:
                r.r[k] = tok
        for w in writes:
            w.w = tok
            w.r = {}

    def op(self, engname, fn, reads=(), writes=(), inc=True, extra=()):
        eng = self.e[engname]
        waits = self._deps(eng, reads, writes, extra)
        if inc:
            eng.count += 1
            tok = (eng.sem, eng.count)
            eng.ops.append((waits, fn, eng.sem, 1))
        else:
            tok = (eng.sem, eng.count + 1)
            eng.ops.append((waits, fn, None, 0))
        self._commit(tok, reads, writes)
        self.n_ops += 1
        return tok

    def dma(self, qname, fn, reads=(), writes=(), extra=()):
        eng = self.e[qname]
        i = eng.dma_i
        eng.dma_i += 1
        k = len(eng.dma_sems)
        sem = eng.dma_sems[i % k]
        ex = list(extra)
        if i // k > 0:
            ex.append((sem, 16 * (i // k)))
        waits = self._deps(eng, reads, writes, ex)
        tok = (sem, 16 * (i // k + 1))
        eng.ops.append((waits, fn, sem, 16))
        self._commit(tok, reads, writes)
        self.n_ops += 1
        return tok

    def dma_cc(self, qname, fn, reads=(), writes=()):
        eng = self.e[qname]
        if not hasattr(self, "cc_sem"):
            raise RuntimeError("no cc semaphore")
        waits = self._deps(eng, reads, writes)
        self.cc_count += 1
        tok = (self.cc_sem, self.cc_count)
        eng.ops.append((waits, fn, self.cc_sem, 1))
        self._commit(tok, reads, writes)
        return tok

    def wait(self, engname, toks):
        eng = self.e[engname]
        waits = self._deps(eng, (), (), toks)
        if waits:
            eng.ops.append((waits, None, None, 0))

    def all_tokens(self):
        toks = []
        for eng in self.e.values():
            if eng.count > 0:
                toks.append((eng.sem, eng.count))
            k = len(eng.dma_sems)
            for j in range(min(eng.dma_i, k)):
                uses = (eng.dma_i - 1 - j) // k + 1
                toks.append((eng.dma_sems[j], 16 * uses))
        return toks

    def barrier(self):
        toks = self.all_tokens()
        for n in self.e:
            self.wait(n, toks)

    def emit(self, block):
        def mk(engname):
            eng = self.e[engname]

            def body(e):
                for waits, fn, isem, amt in eng.ops:
                    for (s, v) in waits:
                        e.wait_ge(s, v)
                    if fn is None:
                        continue
                    ins = fn(e)
                    if isem is self.cc_sem:
                        ins.then_inc(isem)
                    elif isem is not None:
                        ins.then_inc(isem, amt)
            return body
        block.tensor(mk("tensor"))
        block.vector(mk("vector"))
        block.scalar(mk("scalar"))
        block.gpsimd(mk("gpsimd"))
        block.sync(mk("sync"))


def _consts():
    c = {}
    half = 32
    inv = (np.float32(10000.0) ** (-(np.arange(half, dtype=np.float32) / np.float32(half)))).astype(np.float32)
    c["c_invfreq"] = np.concatenate([inv, inv]).reshape(64, 1).astype(np.float32)
    c["c_ident"] = np.eye(128, dtype=np.float32)
    c["c_iota"] = np.tile(np.arange(512, dtype=np.float32)[None, :], (128, 1))
    c["c_tril"] = (np.arange(128)[:, None] < np.arange(128)[None, :]).astype(np.float32)
    t = (np.arange(32)[None, :] * 128 + np.arange(128)[:, None])
    tok = np.stack([t // 64, t % 64, np.ones_like(t)], axis=-1).astype(np.float32)
    c["c_tok"] = tok.reshape(128, 96)
    corr = np.ones((4, 16), np.float32)
    for g, w in enumerate((2, 4, 8, 16)):
        left = w // 2
        right = w - 1 - left
        for i, tt in enumerate(list(range(8)) + list(range(SEQ - 8, SEQ))):
            lo = max(tt - left, 0)
            hi = min(tt + right + 1, SEQ)
            corr[g, i] = w / float(hi - lo)
    c["c_corr"] = np.tile(corr.reshape(1, 64), (128, 1)).astype(np.float32)
    return c


W_SHAPES = {
    "norm_mix": [DEPTH, 1024], "w_in": [DEPTH, 1024, 960], "q_norm": [DEPTH, 256], "kv_norm": [DEPTH, 128],
    "w_uq": [DEPTH, 256, 768], "w_ukv": [DEPTH, 128, 1024], "w_pool": [DEPTH, 4, 128, 128],
    "pool_scale": [DEPTH, 512], "w_out": [DEPTH, 1024, 1024], "norm_x": [DEPTH, 1024], "mem_norm": [DEPTH, 1024],
    "wx_q": [DEPTH, 1024, 1024], "wx_kv": [DEPTH, 1024, 2048], "wx_o": [DEPTH, 1024, 1024],
    "norm_ffn": [DEPTH, 1024], "w_router": [DEPTH, 1024, 16], "w_gate": [DEPTH, 16, 1024, 2048],
    "w_up": [DEPTH, 16, 1024, 2048], "w_down": [DEPTH, 16, 2048, 1024], "norm_final": [1, 1024],
}


BIG_BF16 = ("w_gate", "w_up", "w_down")
BIG = ("w_in", "w_uq", "w_ukv", "w_pool", "w_out", "wx_q", "wx_kv", "wx_o", "w_gate", "w_up", "w_down")


class _Weights:
    def __init__(self, mk):
        self.mk = mk
        self.d = {}
        self.full = {}
        self.res_by_name = {}
        self.prev_bounce = []

    def shape(self, n):
        s = list(W_SHAPES[n])
        if n != "norm_final" and not self.mk.gather:
            s[0] = self.mk.layers
        return s

    def __getitem__(self, n):
        if n in self.d:
            return self.d[n]
        mk = self.mk
        nc = mk.nc
        s = self.shape(n)
        if mk.gather and n in BIG:
            cols = s[-1]
            L = s[0]
            R = int(np.prod(s[1:-1]))
            sh = nc.dram_tensor(n + "_sh", [L * (R // 8), cols], F32, kind="ExternalInput")
            aps = []
            self.full[n] = []
            for l in range(L):
                if n in BIG_BF16:
                    bounce = nc.dram_tensor(f"{n}_bn{l}", [R // 8, cols // 2], F32)
                    full = nc.dram_tensor(f"{n}_full{l}", [R, cols // 2], F32)
                    ap = full.ap().bitcast(BF16)
                else:
                    bounce = nc.dram_tensor(f"{n}_bn{l}", [R // 8, cols], F32)
                    full = nc.dram_tensor(f"{n}_full{l}", [R, cols], F32)
                    ap = full.ap()
                self.res_by_name[f"{n}_full{l}"] = Res()
                self.full[n].append((sh, bounce, full, R // 8))
                if len(s) == 4:
                    ap = ap.rearrange("(e a) b -> e a b", e=s[1])
                aps.append(ap)
            ap = aps
        else:
            ap = nc.dram_tensor(n, s, F32, kind="ExternalInput").ap()
        self.d[n] = ap
        return ap

    def emit_gathers(self):
        mk = self.mk
        for n in BIG:
            self[n]
        for l in range(DEPTH):
            for n in BIG:
                sh, bounce, full, R8 = self.full[n][l]
                r = Res()
                if n in BIG_BF16:
                    cols = sh.shape[1]
                    hc = cols // 2
                    bview = bounce.ap().bitcast(BF16)
                    step = min(R8, 2048)
                    for r0 in range(0, R8, step):
                        r1 = min(R8, r0 + step)
                        for (c0, c1) in ((0, hc), (hc, cols)):
                            tok = mk.S.dma("gpsimd", (lambda eng, bview=bview, sh=sh, r0=r0, r1=r1, l=l, R8=R8, c0=c0,
                                                      c1=c1: eng.dma_start(out=bview[r0:r1, c0:c1],
                                                                           in_=sh[l * R8 + r0:l * R8 + r1, c0:c1])),
                                           [], [r], extra=self.prev_bounce[-3:-2])
                            self.prev_bounce.append(tok)
                else:
                    step = max(1, min(R8, (1 << 22) // sh.shape[1]))
                    for r0 in range(0, R8, step):
                        r1 = min(R8, r0 + step)
                        mk.S.dma("sync", (lambda eng, bounce=bounce, sh=sh, r0=r0, r1=r1, l=l, R8=R8: eng.dma_start(
                            out=bounce[r0:r1, :], in_=sh[l * R8 + r0:l * R8 + r1, :])), [], [r])
                mk.S.dma_cc("gpsimd", (lambda eng, bounce=bounce, full=full: eng.collective_compute(
                    "AllGather", ALU.bypass, replica_groups=[list(range(8))], ins=[bounce.ap().opt()],
                    outs=[full.ap().opt()])), [r], [self.res_by_name[f"{n}_full{l}"]])


class MK:
    def __init__(self, layers=DEPTH, phases="ABCD", final=True, nexp=NEXP, gather=False):
        self.nexp = nexp
        self.gather = gather
        self.layers = layers
        self.phases = phases
        self.final = final
        self.nc = nc = bass.Bass("TRN2", target_bir_lowering=False)
        self.uid = 0
        di = lambda n, s, dt=F32: nc.dram_tensor(n, list(s), dt, kind="ExternalInput").ap()
        self.x_in = di("x", [SEQ, DM])
        self.mem = di("mem", [256, DM])
        self.pos = di("pos", [1, SEQ], I32)
        self.W = _Weights(self)
        self.C = {n: di(n, v.shape) for n, v in _consts().items()}
        self.out = nc.dram_tensor("out", [SEQ, DM], F32, kind="ExternalOutput").ap()
        dint = lambda n, s, dt: nc.dram_tensor(n, list(s), dt, kind="Internal").ap()
        self.xs = dint("xs", [SEQ + 1, DM], F32)
        self.u_d = dint("u_d", [512, SEQ + 16], F32)
        self.qn_d = dint("qn_d", [4, 128, SEQ], BF16)
        self.qr_d = dint("qr_d", [4, 64, SEQ], BF16)
        self.htok_d = dint("htok_d", [SEQ + 1, DM], BF16)
        self.cos_d = dint("cos_d", [64, SEQ], F32)
        self.sin_d = dint("sin_d", [64, SEQ], F32)
        self.r_xs = Res()
        self.r_ud = Res()
        self.r_q = Res()
        self.r_htok = Res()
        self.r_cs = Res()
        self.r_out = Res()
        self.r_wfull = Res()

    def sbt(self, st, name, shape, dt):
        self.uid += 1
        return st.enter_context(self.nc.sbuf_tensor(f"{name}_{self.uid}", list(shape), dt))

    def mm(self, out, lhsT, rhs, start, stop, reads, writes, inc=True):
        return self.S.op("tensor", lambda e: e.matmul(out, lhsT=lhsT, rhs=rhs, start=start, stop=stop),
                         reads, writes, inc)

    def tr(self, out, in_, ident, reads, writes, inc=True):
        return self.S.op("tensor", lambda e: e.transpose(out, in_, ident), reads, writes, inc)

    def act(self, out, in_, func, reads, writes, **kw):
        return self.S.op("scalar", lambda e: e.activation(out=out, in_=in_, func=func, **kw), reads, writes)

    def vec(self, method, reads, writes, **kw):
        return self.S.op("vector", lambda e: getattr(e, method)(**kw), reads, writes)

    def pool(self, method, reads, writes, **kw):
        return self.S.op("gpsimd", lambda e: getattr(e, method)(**kw), reads, writes)

    def dma(self, q, out, in_, reads, writes, **kw):
        if self.gather:
            nm = getattr(getattr(in_, "tensor", None), "name", None)
            r = self.W.res_by_name.get(nm)
            if r is not None:
                reads = list(reads) + [r]
        return self.S.dma(q, lambda e: e.dma_start(out=out, in_=in_, **kw), reads, writes)

    def build(self):
        nc = self.nc
        with contextlib.ExitStack() as st:
            self.S = Sched(nc, st)
            self.ps = [st.enter_context(nc.psum_tensor(f"ps{i}", [128, 512], F32)) for i in range(8)]
            self.psr = [Res() for _ in range(8)]
            self.ident_b = self.sbt(st, "ident_b", [128, 128], BF16)
            self.ident_f = self.sbt(st, "ident_f", [128, 128], F32)
            self.ones_b = self.sbt(st, "ones_b", [128, 128], BF16)
            self.r_const = Res()
            self.dma("gpsimd", self.ident_b[:], self.C["c_ident"], [], [self.r_const])
            self.dma("sync", self.ident_f[:], self.C["c_ident"], [], [self.r_const])
            self.vec("memset", [], [self.r_const], ap=self.ones_b[:], constant=1.0)
            self.cst = self.sbt(st, "cst", [128, 8], F32)
            self.vec("memset", [], [self.r_const], ap=self.cst[:, 0:1], constant=EPS)
            self.vec("memset", [], [self.r_const], ap=self.cst[:, 1:2], constant=float(np.pi / 2))
            self.vec("memset", [], [self.r_const], ap=self.cst[:, 2:3], constant=0.0)
            self.eps_t = self.cst
            if self.gather:
                self.W.emit_gathers()
                self.S.barrier()
            self.phase_init()
            self.phase_rope()
            x_src = self.x_in
            for l in range(self.layers):
                if "A" in self.phases:
                    self.phase_AB(l, x_src, self.xs)
                    x_src = self.xs
                if "C" in self.phases:
                    self.phase_C(l, x_src, self.xs)
                    x_src = self.xs
                if "D" in self.phases:
                    self.phase_D(l, x_src)
                    x_src = self.xs
            self.phase_F(x_src)
            self.S.barrier()
            with nc.Block() as block:
                self.S.emit(block)
        return nc

    def phase_init(self):
        with contextlib.ExitStack() as st:
            z = self.sbt(st, "zrow", [1, DM], BF16)
            rz = Res()
            self.vec("memset", [], [rz], ap=z[:], constant=0.0)
            self.dma("sync", self.htok_d[SEQ:SEQ + 1, :], z[:], [rz], [self.r_htok])
            if "A" not in self.phases and "C" not in self.phases:
                xt = self.sbt(st, "xcp", [128, 4, DM], F32)
                rx = Res()
                for i in range(NT):
                    rows = self.x_in[i * 512:(i + 1) * 512, :].rearrange("(j p) d -> p j d", p=128)
                    drows = self.xs[i * 512:(i + 1) * 512, :].rearrange("(j p) d -> p j d", p=128)
                    self.dma("sync", xt[:], rows, [], [rx])
                    self.dma("sync", drows, xt[:], [rx], [self.r_xs])
        self.S.barrier()

    def norm_tile(self, xt, r_xt, nsub, gain_b, r_gain, hout, r_hout, junk, r_junk, stat, r_stat):
        ssq = stat[:, 0:nsub]
        lnv = stat[:, 8:8 + nsub]
        rstd = stat[:, 16:16 + nsub]
        for j in range(nsub):
            self.act(junk[:], xt[:, j, :], AF.Square, [r_xt], [r_junk, r_stat], accum_out=stat[:, j:j + 1])
        self.act(lnv, ssq, AF.Ln, [r_stat, self.r_const], [r_stat], scale=1.0 / DM, bias=self.eps_t[:, 0:1])
        self.act(rstd, lnv, AF.Exp, [r_stat], [r_stat], scale=-0.5)
        for j in range(nsub):
            self.vec("scalar_tensor_tensor", [r_xt, r_stat, r_gain], [r_hout], out=hout[:, j, :], in0=xt[:, j, :],
                     scalar=stat[:, 16 + j:17 + j], in1=gain_b[:], op0=ALU.mult, op1=ALU.mult)

    def transpose_tile(self, hb, r_hb, nsub, hT, r_hT, banks, dt=BF16, ident=None):
        ident = self.ident_b if ident is None else ident
        w = nsub * 128
        for k in range(8):
            b = banks[k % len(banks)]
            pv = self.ps[b][:].bitcast(BF16) if dt == BF16 else self.ps[b][:]
            for j in range(nsub):
                self.tr(pv[:, j * 128:(j + 1) * 128], hb[:, j, k * 128:(k + 1) * 128], ident[:],
                        [r_hb, self.r_const], [self.psr[b]], inc=(j == nsub - 1))
            if k % 2 == 0:
                self.act(hT[:, k, :], pv[:, 0:w], AF.Copy, [self.psr[b]], [r_hT])
            else:
                self.vec("tensor_copy", [self.psr[b]], [r_hT], out=hT[:, k, :], in_=pv[:, 0:w])

    def load_gain_b(self, st, name, src_row):
        g = self.sbt(st, name, [128, DM], F32)
        r = Res()
        self.dma("sync", g[:], src_row.to_broadcast([128, DM]), [], [r])
        return g, r

    def load_w(self, st, name, src, K, cols):
        w = self.sbt(st, name, [128, K, cols], BF16)
        r = Res()
        for k in range(K):
            self.dma("gpsimd", w[:, k, :], src[k * 128:(k + 1) * 128, :], [], [r])
        return w, r

    def phase_rope(self):
        if "A" not in self.phases:
            return
        TWO_PI = float(2 * np.pi)
        C1 = 6.28125
        C2 = TWO_PI - C1
        MAGIC = 12582912.0
        PI_LO = 3.1415925
        with contextlib.ExitStack() as st:
            invf = self.sbt(st, "invf", [64, 1], F32)
            r_invf = Res()
            self.dma("sync", invf[:], self.C["c_invfreq"], [], [r_invf])
            W = 1024
            posi = self.sbt(st, "posi", [64, W], I32)
            a = self.sbt(st, "ra", [64, W], F32)
            b = self.sbt(st, "rb", [64, W], F32)
            c = self.sbt(st, "rc", [64, W], F32)
            sn = self.sbt(st, "rsn", [64, W], F32)
            cs = self.sbt(st, "rcs", [64, W], F32)
            r1, ra, rb, rc, rsn, rcs = Res(), Res(), Res(), Res(), Res(), Res()
            for ch in range(SEQ // W):
                sl = slice(ch * W, (ch + 1) * W)
                self.dma("sync", posi[:], self.pos[0:1, sl].to_broadcast([64, W]), [], [r1])
                self.vec("tensor_copy", [r1], [ra], out=a[:], in_=posi[:])
                self.vec("tensor_scalar", [ra, r_invf], [ra], out=a[:], in0=a[:], scalar1=invf[:, 0:1], scalar2=None,
                         op0=ALU.mult)
                self.vec("tensor_scalar", [ra], [rb], out=b[:], in0=a[:], scalar1=1.0 / TWO_PI, scalar2=MAGIC,
                         op0=ALU.mult, op1=ALU.add)
                self.vec("tensor_scalar", [rb], [rb], out=b[:], in0=b[:], scalar1=-MAGIC, scalar2=None, op0=ALU.add)
                self.vec("scalar_tensor_tensor", [rb, ra], [rc], out=c[:], in0=b[:], scalar=-C1, in1=a[:],
                         op0=ALU.mult, op1=ALU.add)
                self.vec("scalar_tensor_tensor", [rb, rc], [rc], out=c[:], in0=b[:], scalar=-C2, in1=c[:],
                         op0=ALU.mult, op1=ALU.add)
                self.vec("tensor_scalar", [rc], [rc], out=c[:], in0=c[:], scalar1=-PI_LO, scalar2=PI_LO,
                         op0=ALU.max, op1=ALU.min)
                self.act(sn[:], c[:], AF.Sin, [rc], [rsn])
                self.vec("scalar_tensor_tensor", [rc], [rb], out=b[:], in0=c[:], scalar=-1.0, in1=c[:],
                         op0=ALU.mult, op1=ALU.max)
                self.act(cs[:], b[:], AF.Sin, [rb, self.r_const], [rcs], scale=-1.0, bias=self.cst[0:64, 1:2])
                self.dma("sync", self.sin_d[:, sl], sn[:], [rsn], [self.r_cs])
                self.dma("sync", self.cos_d[:, sl], cs[:], [rcs], [self.r_cs])
        self.S.barrier()

    def phase_F(self, x_src):
        with contextlib.ExitStack() as st:
            gain, r_gain = self.load_gain_b(st, "gF", self.W["norm_final"][0:1, :])
            xt = [self.sbt(st, "xtF", [128, 4, DM], F32) for _ in range(2)]
            ho = [self.sbt(st, "hoF", [128, 4, DM], F32) for _ in range(2)]
            junk = self.sbt(st, "junkF", [128, DM], BF16)
            stat = [self.sbt(st, "statF", [128, 24], F32) for _ in range(2)]
            r_xt = [Res(), Res()]
            r_ho = [Res(), Res()]
            r_stat = [Res(), Res()]
            r_junk = Res()
            for i in range(NT):
                p = i % 2
                rows = x_src[i * 512:(i + 1) * 512, :].rearrange("(j p) d -> p j d", p=128)
                orows = self.out[i * 512:(i + 1) * 512, :].rearrange("(j p) d -> p j d", p=128)
                self.dma("sync", xt[p][:], rows, [self.r_xs], [r_xt[p]])
                if self.final:
                    self.norm_tile(xt[p], r_xt[p], 4, gain, r_gain, ho[p], r_ho[p], junk, r_junk, stat[p], r_stat[p])
                    self.dma("sync", orows, ho[p][:], [r_ho[p]], [self.r_out])
                else:
                    self.dma("sync", orows, xt[p][:], [r_xt[p]], [self.r_out])
        self.S.barrier()

    def phase_C(self, l, x_src, x_dst):
        W = self.W
        sc = 1.0 / 16.0
        with contextlib.ExitStack() as st:
            wq, r_wq = self.load_w(st, "wq", W["wx_q"][l], 8, 1024)
            wkv, r_wkv = self.load_w(st, "wkv", W["wx_kv"][l], 8, 2048)
            wo, r_wo = self.load_w(st, "wo", W["wx_o"][l], 8, 1024)
            gx, r_gx = self.load_gain_b(st, "gx", W["norm_x"][l:l + 1, :])
            gm, r_gm = self.load_gain_b(st, "gm", W["mem_norm"][l:l + 1, :])
            junk = self.sbt(st, "junk", [128, DM], BF16)
            r_junk = Res()
            mt = self.sbt(st, "mt", [128, 2, DM], F32)
            mb = self.sbt(st, "mb", [128, 2, DM], BF16)
            mT = self.sbt(st, "mT", [128, 8, 256], BF16)
            KxT = self.sbt(st, "KxT", [128, 8, 256], BF16)
            Vx = self.sbt(st, "Vx", [128, 2, DM], BF16)
            mstat = self.sbt(st, "mstat", [128, 24], F32)
            r_mt, r_mb, r_mT, r_K, r_V, r_ms = Res(), Res(), Res(), Res(), Res(), Res()
            self.dma("sync", mt[:], self.mem.rearrange("(j p) d -> p j d", p=128), [], [r_mt])
            self.norm_tile(mt, r_mt, 2, gm, r_gm, mb, r_mb, junk, r_junk, mstat, r_ms)
            self.transpose_tile(mb, r_mb, 2, mT, r_mT, [0, 1])
            for c in range(8):
                b = 2 + c % 2
                for k in range(8):
                    self.mm(self.ps[b][:, 0:256], wkv[:, k, c * 128:(c + 1) * 128], mT[:, k, :], k == 0, k == 7,
                            [r_wkv, r_mT], [self.psr[b]], inc=(k == 7))
                self.act(KxT[:, c, :], self.ps[b][:, 0:256], AF.Copy, [self.psr[b]], [r_K])
            for j in range(2):
                for hf in range(2):
                    b = 4 + (j * 2 + hf) % 2
                    for k in range(8):
                        self.mm(self.ps[b][:], mT[:, k, j * 128:(j + 1) * 128],
                                wkv[:, k, 1024 + hf * 512:1024 + (hf + 1) * 512], k == 0, k == 7,
                                [r_wkv, r_mT], [self.psr[b]], inc=(k == 7))
                    self.act(Vx[:, j, hf * 512:(hf + 1) * 512], self.ps[b][:], AF.Copy, [self.psr[b]], [r_V])
            xt = [self.sbt(st, "xt", [128, 4, DM], F32) for _ in range(2)]
            r_xt = [Res(), Res()]
            hb = self.sbt(st, "hb", [128, 4, DM], BF16)
            hT = self.sbt(st, "hT", [128, 8, 512], BF16)
            qT = self.sbt(st, "qT", [128, 8, 512], BF16)
            oT = self.sbt(st, "oT", [128, 8, 512], BF16)
            PT = [self.sbt(st, "PT", [128, 2, 512], BF16) for _ in range(2)]
            rs = [self.sbt(st, "rs", [128, 512], F32) for _ in range(2)]
            stat = self.sbt(st, "stat", [128, 24], F32)
            r_hb, r_hT, r_qT, r_oT, r_stat = Res(), Res(), Res(), Res(), Res()
            r_PT = [Res(), Res()]
            r_rs = [Res(), Res()]
            for i in range(NT):
                p = i % 2
                rows = x_src[i * 512:(i + 1) * 512, :].rearrange("(j p) d -> p j d", p=128)
                drows = x_dst[i * 512:(i + 1) * 512, :].rearrange("(j p) d -> p j d", p=128)
                self.dma("sync", xt[p][:], rows, [self.r_xs], [r_xt[p]])
                self.norm_tile(xt[p], r_xt[p], 4, gx, r_gx, hb, r_hb, junk, r_junk, stat, r_stat)
                self.transpose_tile(hb, r_hb, 4, hT, r_hT, [0, 1])
                for c in range(8):
                    b = 2 + c % 2
                    for k in range(8):
                        self.mm(self.ps[b][:], wq[:, k, c * 128:(c + 1) * 128], hT[:, k, :], k == 0, k == 7,
                                [r_wq, r_hT], [self.psr[b]], inc=(k == 7))
                    self.act(qT[:, c, :], self.ps[b][:], AF.Copy, [self.psr[b]], [r_qT])
                for h in range(4):
                    hp = h % 2
                    for mj in range(2):
                        b = 4 + mj
                        for dc in range(2):
                            self.mm(self.ps[b][:], KxT[:, h * 2 + dc, mj * 128:(mj + 1) * 128], qT[:, h * 2 + dc, :],
                                    dc == 0, dc == 1, [r_K, r_qT], [self.psr[b]], inc=(dc == 1))
                        self.act(PT[hp][:, mj, :], self.ps[b][:], AF.Exp, [self.psr[b]], [r_PT[hp]], scale=sc)
                    b = 6
                    for mj in range(2):
                        self.mm(self.ps[b][:], self.ones_b[:], PT[hp][:, mj, :], mj == 0, mj == 1,
                                [r_PT[hp], self.r_const], [self.psr[b]], inc=(mj == 1))
                    self.vec("reciprocal", [self.psr[b]], [r_rs[hp]], out=rs[hp][:], in_=self.ps[b][:])
                    for dvc in range(2):
                        b = 7 if dvc == 0 else 0
                        for mj in range(2):
                            self.mm(self.ps[b][:], Vx[:, mj, h * 256 + dvc * 128:h * 256 + (dvc + 1) * 128],
                                    PT[hp][:, mj, :], mj == 0, mj == 1, [r_V, r_PT[hp]], [self.psr[b]], inc=(mj == 1))
                        self.vec("tensor_tensor", [self.psr[b], r_rs[hp]], [r_oT], out=oT[:, h * 2 + dvc, :],
                                 in0=self.ps[b][:], in1=rs[hp][:], op=ALU.mult)
                for j in range(4):
                    for hf in range(2):
                        b = 1 + (j * 2 + hf) % 3
                        for c in range(8):
                            self.mm(self.ps[b][:], oT[:, c, j * 128:(j + 1) * 128], wo[:, c, hf * 512:(hf + 1) * 512],
                                    c == 0, c == 7, [r_oT, r_wo], [self.psr[b]], inc=(c == 7))
                        self.vec("tensor_tensor", [self.psr[b], r_xt[p]], [r_xt[p]],
                                 out=xt[p][:, j, hf * 512:(hf + 1) * 512], in0=self.ps[b][:],
                                 in1=xt[p][:, j, hf * 512:(hf + 1) * 512], op=ALU.add)
                self.dma("sync", drows, xt[p][:], [r_xt[p]], [self.r_xs])
        self.S.barrier()


def make_in_maps(mk, inputs, n_cores=8):
    consts = _consts()
    maps = []
    names = list(mk.W.d.keys())
    for b in range(n_cores):
        m = {"x": np.ascontiguousarray(inputs["x"][b]), "mem": np.ascontiguousarray(inputs["mem"][b]),
             "pos": np.ascontiguousarray(np.asarray(inputs["positions"][b]).reshape(1, SEQ).astype(np.int32))}
        for n in names:
            s = mk.W.shape(n)
            a = np.asarray(inputs[n], dtype=np.float32)
            if n == "norm_final":
                a = a.reshape(1, DM)
            if mk.gather and n in BIG:
                a3 = a.reshape(s[0], -1, s[-1])
                r8 = a3.shape[1] // 8
                m[n + "_sh"] = np.ascontiguousarray(a3[:, b * r8:(b + 1) * r8, :]).reshape(s[0] * r8, s[-1])
            else:
                m[n] = np.ascontiguousarray(a[:s[0]] if n != "norm_final" else a)
        m.update(consts)
        maps.append(m)
    return maps


_CACHE = {}


def kernel(**inputs):
    if "mk" not in _CACHE:
        mk = MK(gather=True)
        mk.build()
        _CACHE["mk"] = mk
    mk = _CACHE["mk"]
    maps = make_in_maps(mk, inputs, 8)
    res = run_bass_kernel_spmd(mk.nc, maps, core_ids=list(range(8)))
    return np.stack([np.asarray(r["out"]) for r in res.results], axis=0).astype(np.float32)


def _phase_A(self, l, x_src, st):
    W = self.W
    win = self.sbt(st, "win", [128, 8, 1024], BF16)
    r_win = Res()
    for k in range(8):
        rows = W["w_in"][l][k * 128:(k + 1) * 128, :]
        self.dma("gpsimd", win[:, k, 0:960], rows, [], [r_win])
        self.dma("gpsimd", win[:, k, 960:992], rows[:, 928:960], [], [r_win])
        self.dma("gpsimd", win[:, k, 992:1024], rows[:, 896:928], [], [r_win])
    self.vec("tensor_scalar", [r_win], [r_win], out=win[:, :, 960:992], in0=win[:, :, 960:992], scalar1=-1.0,
             scalar2=None, op0=ALU.mult)
    wuq = self.sbt(st, "wuq", [128, 2, 1024], BF16)
    r_wuq = Res()
    for k in range(2):
        rows = W["w_uq"][l][k * 128:(k + 1) * 128, :]
        self.dma("gpsimd", wuq[:, k, 0:768], rows, [], [r_wuq])
        for h in range(4):
            c0 = h * 192 + 128
            self.dma("gpsimd", wuq[:, k, 768 + h * 64:768 + h * 64 + 32], rows[:, c0 + 32:c0 + 64], [], [r_wuq])
            self.dma("gpsimd", wuq[:, k, 768 + h * 64 + 32:768 + h * 64 + 64], rows[:, c0:c0 + 32], [], [r_wuq])
    for h in range(4):
        self.vec("tensor_scalar", [r_wuq], [r_wuq], out=wuq[:, :, 768 + h * 64:768 + h * 64 + 32],
                 in0=wuq[:, :, 768 + h * 64:768 + h * 64 + 32], scalar1=-1.0, scalar2=None, op0=ALU.mult)
    wukv = self.sbt(st, "wukv", [128, 1024], BF16)
    r_wukv = Res()
    self.dma("gpsimd", wukv[:], W["w_ukv"][l], [], [r_wukv])
    gmix, r_gmix = self.load_gain_b(st, "gmix", W["norm_mix"][l:l + 1, :])
    gq = self.sbt(st, "gq", [128, 2, 1], F32)
    gkv = self.sbt(st, "gkv", [128, 1, 1], F32)
    r_g = Res()
    self.dma("sync", gq[:], W["q_norm"][l].rearrange("(k p o) -> p k o", p=128, o=1), [], [r_g], allow_slow_non_contiguous=True)
    self.dma("sync", gkv[:], W["kv_norm"][l].rearrange("(k p o) -> p k o", p=128, o=1), [], [r_g], allow_slow_non_contiguous=True)
    xt = [self.sbt(st, "xtA", [128, 4, DM], F32) for _ in range(2)]
    r_xt = [Res(), Res()]
    hb = self.sbt(st, "hbA", [128, 4, DM], BF16)
    hT = self.sbt(st, "hTA", [128, 8, 512], BF16)
    junk = self.sbt(st, "junkA", [128, DM], BF16)
    stat = self.sbt(st, "statA", [128, 24], F32)
    uT0 = self.sbt(st, "uT", [128, 4, 512], F32)
    uT = [uT0, uT0]
    r_uT0 = Res()
    r_uT = [r_uT0, r_uT0]
    cq = self.sbt(st, "cq", [128, 3, 512], F32)
    sq = self.sbt(st, "sq", [128, 3, 512], BF16)
    rstd = self.sbt(st, "rstdA", [128, 2, 512], F32)
    cn = self.sbt(st, "cn", [128, 3, 512], BF16)
    cst = [self.sbt(st, "cosA", [64, 2, 512], F32) for _ in range(2)]
    r_cst = [Res(), Res()]
    t1 = self.sbt(st, "ropt1", [64, 512], F32)
    t2 = self.sbt(st, "ropt2", [64, 512], F32)
    qn0 = self.sbt(st, "qnA", [128, 4, 512], BF16)
    qr0 = self.sbt(st, "qrA", [64, 4, 512], BF16)
    qn, qr = [qn0, qn0], [qr0, qr0]
    r_qn0, r_qr0 = Res(), Res()
    r_qn, r_qr = [r_qn0, r_qn0], [r_qr0, r_qr0]
    r_hb, r_hT, r_junk, r_stat, r_cq, r_sq, r_rstd, r_cn, r_t1, r_t2 = (Res() for _ in range(10))
    KT, krT, V = self.KT, self.krT, self.V

    def rope_combine(ps_a, ps_b, b_a, b_b, cs, r_cs, out_ap, r_out):
        self.vec("tensor_tensor", [self.psr[b_a], r_cs], [r_t1], out=t1[:], in0=ps_a, in1=cs[:, 0, :], op=ALU.mult)
        self.vec("tensor_tensor", [self.psr[b_b], r_cs], [r_t2], out=t2[:], in0=ps_b, in1=cs[:, 1, :], op=ALU.mult)
        self.vec("tensor_tensor", [r_t1, r_t2], [r_out], out=out_ap, in0=t1[:], in1=t2[:], op=ALU.add)

    for i in range(NT):
        p = i % 2
        cols = slice(i * 512, (i + 1) * 512)
        rows = x_src[i * 512:(i + 1) * 512, :].rearrange("(j p) d -> p j d", p=128)
        self.dma("sync", xt[p][:], rows, [self.r_xs], [r_xt[p]])
        self.dma("sync", cst[p][:, 0, :], self.cos_d[:, cols], [self.r_cs], [r_cst[p]])
        self.dma("sync", cst[p][:, 1, :], self.sin_d[:, cols], [self.r_cs], [r_cst[p]])
        self.norm_tile(xt[p], r_xt[p], 4, gmix, r_gmix, hb, r_hb, junk, r_junk, stat, r_stat)
        self.transpose_tile(hb, r_hb, 4, hT, r_hT, [0, 1])
        for g in range(4):
            b = 2 + g % 2
            for k in range(8):
                self.mm(self.ps[b][:], win[:, k, g * 128:(g + 1) * 128], hT[:, k, :], k == 0, k == 7,
                        [r_win, r_hT], [self.psr[b]], inc=(k == 7))
            self.act(uT[p][:, g, :], self.ps[b][:], AF.Copy, [self.psr[b]], [r_uT[p]])
        self.dma("sync", self.u_d[:, 8 + i * 512:8 + (i + 1) * 512].rearrange("(g p) t -> p g t", p=128), uT[p][:],
                 [r_uT[p]], [self.r_ud])
        for c in range(3):
            b = 4 + c % 2
            for k in range(8):
                self.mm(self.ps[b][:], win[:, k, 512 + c * 128:512 + (c + 1) * 128], hT[:, k, :], k == 0, k == 7,
                        [r_win, r_hT], [self.psr[b]], inc=(k == 7))
            self.act(cq[:, c, :], self.ps[b][:], AF.Copy, [self.psr[b]], [r_cq])
            self.act(sq[:, c, :], self.ps[b][:], AF.Square, [self.psr[b]], [r_sq])
        for which in range(2):
            b = 6
            ks = (0, 1) if which == 0 else (2,)
            n = 256.0 if which == 0 else 128.0
            for ii, c in enumerate(ks):
                self.mm(self.ps[b][:], self.ones_b[:], sq[:, c, :], ii == 0, ii == len(ks) - 1,
                        [r_sq, self.r_const], [self.psr[b]], inc=(ii == len(ks) - 1))
            self.act(rstd[:, which, :], self.ps[b][:], AF.Ln, [self.psr[b], self.r_const], [r_rstd], scale=1.0 / n,
                     bias=self.cst[:, 0:1])
            self.act(rstd[:, which, :], rstd[:, which, :], AF.Exp, [r_rstd], [r_rstd], scale=-0.5)
        for c in range(3):
            gsc = gq[:, c, :] if c < 2 else gkv[:, 0, :]
            self.vec("scalar_tensor_tensor", [r_cq, r_rstd, r_g], [r_cn], out=cn[:, c, :], in0=cq[:, c, :], scalar=gsc,
                     in1=rstd[:, 0 if c < 2 else 1, :], op0=ALU.mult, op1=ALU.mult)
        for which in range(2):
            b = 7 if which == 0 else 0
            c0 = 896 if which == 0 else 960
            for k in range(8):
                self.mm(self.ps[b][0:64, :], win[:, k, c0:c0 + 64], hT[:, k, :], k == 0, k == 7,
                        [r_win, r_hT], [self.psr[b]], inc=(k == 7))
        rope_combine(self.ps[7][0:64, :], self.ps[0][0:64, :], 7, 0, cst[p], r_cst[p], krT[0:64, cols], self.r_krT)
        for h in range(4):
            b = 1
            for k in range(2):
                self.mm(self.ps[b][:], wuq[:, k, h * 192:h * 192 + 128], cn[:, k, :], k == 0, k == 1,
                        [r_wuq, r_cn], [self.psr[b]], inc=(k == 1))
            self.act(qn[p][:, h, :], self.ps[b][:], AF.Copy, [self.psr[b]], [r_qn[p]])
            for which in range(2):
                b = 2 + which
                c0 = h * 192 + 128 if which == 0 else 768 + h * 64
                for k in range(2):
                    self.mm(self.ps[b][0:64, :], wuq[:, k, c0:c0 + 64], cn[:, k, :], k == 0, k == 1,
                            [r_wuq, r_cn], [self.psr[b]], inc=(k == 1))
            rope_combine(self.ps[2][0:64, :], self.ps[3][0:64, :], 2, 3, cst[p], r_cst[p], qr[p][:, h, :], r_qr[p])
        self.dma("sync", self.qn_d[:, :, cols].rearrange("h p t -> p h t"), qn[p][:], [r_qn[p]], [self.r_q])
        self.dma("sync", self.qr_d[:, :, cols].rearrange("h p t -> p h t"), qr[p][:], [r_qr[p]], [self.r_q])
        for h in range(4):
            b = 4 + h % 2
            self.mm(self.ps[b][:], wukv[:, h * 256:h * 256 + 128], cn[:, 2, :], True, True, [r_wukv, r_cn],
                    [self.psr[b]])
            self.act(KT[:, h, cols], self.ps[b][:], AF.Copy, [self.psr[b]], [self.r_KT])
        for j in range(4):
            b = 6 + j % 2
            for h in range(4):
                self.mm(self.ps[b][:, h * 128:(h + 1) * 128], cn[:, 2, j * 128:(j + 1) * 128],
                        wukv[:, h * 256 + 128:h * 256 + 256], True, True, [r_wukv, r_cn], [self.psr[b]], inc=(h == 3))
            self.vec("tensor_copy", [self.psr[b]], [self.r_V], out=V[:, i * 4 + j, :], in_=self.ps[b][:])


def _phase_B(self, l, x_src, x_dst, st):
    W = self.W
    sc = float(192.0 ** -0.5)
    wout, r_wout = self.load_w(st, "wout", W["w_out"][l], 8, 1024)
    wpool = self.sbt(st, "wpool", [128, 4, 128], BF16)
    r_wpool = Res()
    for g in range(4):
        self.dma("gpsimd", wpool[:, g, :], W["w_pool"][l][g], [], [r_wpool])
    pscale = self.sbt(st, "pscale", [128, 4, 1], F32)
    corr = self.sbt(st, "corr", [128, 64], F32)
    r_ps = Res()
    self.dma("sync", pscale[:], W["pool_scale"][l].rearrange("(g p o) -> p g o", p=128, o=1), [], [r_ps], allow_slow_non_contiguous=True)
    self.dma("sync", corr[:], self.C["c_corr"], [], [r_ps])
    xt = [self.sbt(st, "xtB", [128, 4, DM], F32) for _ in range(2)]
    r_xt = [Res(), Res()]
    U = [self.sbt(st, "U", [128, 4, 528], F32) for _ in range(2)]
    r_U = [Res(), Res()]
    pa = self.sbt(st, "pa", [128, 528], F32)
    pb = self.sbt(st, "pb", [128, 528], F32)
    r_pa, r_pb = Res(), Res()
    diff = self.sbt(st, "diff", [128, 4, 512], BF16)
    r_diff = Res()
    mixT = self.sbt(st, "mixT", [128, 8, 512], BF16)
    r_mix = Res()
    qn = [self.sbt(st, "qnB", [128, 4, 512], BF16) for _ in range(2)]
    qr = [self.sbt(st, "qrB", [64, 4, 512], BF16) for _ in range(2)]
    r_qb = [Res(), Res()]
    NP = 6
    ones_f = self.sbt(st, "ones_f", [128, 128], F32)
    r_onesf = Res()
    self.vec("memset", [], [r_onesf], ap=ones_f[:], constant=1.0)
    accS = [self.sbt(st, "accS", [128, 512], F32) for _ in range(2)]
    r_acc = [Res(), Res()]
    PT = [self.sbt(st, "PTB", [128, 512], BF16) for _ in range(NP)]
    r_PT = [Res() for _ in range(NP)]
    rs = self.sbt(st, "rsB", [128, 512], F32)
    r_rs = Res()
    KT, krT, V = self.KT, self.krT, self.V
    pti = 0
    for i in range(NT):
        p = i % 2
        cols = slice(i * 512, (i + 1) * 512)
        rows = x_src[i * 512:(i + 1) * 512, :].rearrange("(j p) d -> p j d", p=128)
        drows = x_dst[i * 512:(i + 1) * 512, :].rearrange("(j p) d -> p j d", p=128)
        self.dma("sync", xt[p][:], rows, [self.r_xs], [r_xt[p]])
        self.dma("sync", U[p][:], self.u_d[:, i * 512:i * 512 + 528].rearrange("(g p) t -> p g t", p=128),
                 [self.r_ud], [r_U[p]])
        self.dma("sync", qn[p][:], self.qn_d[:, :, cols].rearrange("h p t -> p h t"), [self.r_q], [r_qb[p]])
        self.dma("sync", qr[p][:], self.qr_d[:, :, cols].rearrange("h p t -> p h t"), [self.r_q], [r_qb[p]])
        for g, w in enumerate((2, 4, 8, 16)):
            right = w - 1 - w // 2
            src, r_src = U[p][:, g, :], r_U[p]
            bufs = [(pa, r_pa), (pb, r_pb)]
            d = 1
            bi = 0
            while 2 * d <= w:
                dst, r_dst = bufs[bi]
                self.pool("tensor_tensor", [r_src], [r_dst], out=dst[:, d:528], in0=src[:, d:528],
                          in1=src[:, 0:528 - d], op=ALU.add)
                src, r_src = dst, r_dst
                bi ^= 1
                d *= 2
            if i == 0:
                self.pool("tensor_tensor", [r_src, r_ps], [r_src], out=src[:, 8 + right:16 + right],
                          in0=src[:, 8 + right:16 + right], in1=corr[:, g * 16:g * 16 + 8], op=ALU.mult)
            if i == NT - 1:
                self.pool("tensor_tensor", [r_src, r_ps], [r_src], out=src[:, 512 + right:520 + right],
                          in0=src[:, 512 + right:520 + right], in1=corr[:, g * 16 + 8:g * 16 + 16], op=ALU.mult)
            self.vec("scalar_tensor_tensor", [r_src, r_U[p]], [r_diff], out=diff[:, g, :],
                     in0=src[:, 8 + right:520 + right], scalar=1.0 / w, in1=U[p][:, g, 8:520],
                     op0=ALU.mult, op1=ALU.subtract)
            b = 7
            self.mm(self.ps[b][:], wpool[:, g, :], diff[:, g, :], True, True, [r_wpool, r_diff], [self.psr[b]])
            self.vec("tensor_scalar", [self.psr[b], r_ps], [r_mix], out=mixT[:, g, :], in0=self.ps[b][:],
                     scalar1=pscale[:, g, :], scalar2=None, op0=ALU.mult)
        steps = [(h, kc) for h in range(4) for kc in range(32)]

        def emit_qk(sidx):
            h, kc = steps[sidx]
            b = sidx % 3
            ks = slice(kc * 128, (kc + 1) * 128)
            self.mm(self.ps[b][:], KT[:, h, ks], qn[p][:, h, :], True, False, [self.r_KT, r_qb[p]],
                    [self.psr[b]], inc=False)
            self.mm(self.ps[b][:], krT[0:64, ks], qr[p][:, h, :], False, True, [self.r_krT, r_qb[p]],
                    [self.psr[b]])

        emit_qk(0)
        emit_qk(1)
        for sidx, (h, kc) in enumerate(steps):
            bo = 3 + h % 2
            bs = 5 + h % 2
            b = sidx % 3
            pt, r_pt = PT[pti % NP], r_PT[pti % NP]
            pti += 1
            self.act(pt[:], self.ps[b][:], AF.Exp, [self.psr[b]], [r_pt], scale=sc)
            if sidx + 2 < len(steps):
                emit_qk(sidx + 2)
            last = kc == 31
            self.mm(self.ps[bo][:], V[:, kc, h * 128:(h + 1) * 128], pt[:], kc == 0, last, [self.r_V, r_pt],
                    [self.psr[bo]], inc=True)
            ai = kc % 2
            emit = self.vec if ai == 0 else self.pool
            if kc < 2:
                emit("tensor_copy", [r_pt], [r_acc[ai]], out=accS[ai][:], in_=pt[:])
            else:
                emit("tensor_tensor", [r_pt, r_acc[ai]], [r_acc[ai]], out=accS[ai][:], in0=accS[ai][:], in1=pt[:],
                     op=ALU.add)
            if last:
                self.vec("tensor_tensor", [r_acc[0], r_acc[1]], [r_acc[0]], out=accS[0][:], in0=accS[0][:],
                         in1=accS[1][:], op=ALU.add)
                self.mm(self.ps[bs][:], ones_f[:], accS[0][:], True, True, [r_onesf, r_acc[0]], [self.psr[bs]])
                self.vec("reciprocal", [self.psr[bs]], [r_rs], out=rs[:], in_=self.ps[bs][:])
                self.vec("tensor_tensor", [self.psr[bo], r_rs], [r_mix], out=mixT[:, 4 + h, :], in0=self.ps[bo][:],
                         in1=rs[:], op=ALU.mult)
        for j in range(4):
            for hf in range(2):
                b = (j * 2 + hf) % 3
                for c in range(8):
                    self.mm(self.ps[b][:], mixT[:, c, j * 128:(j + 1) * 128], wout[:, c, hf * 512:(hf + 1) * 512],
                            c == 0, c == 7, [r_mix, r_wout], [self.psr[b]], inc=(c == 7))
                self.vec("tensor_tensor", [self.psr[b], r_xt[p]], [r_xt[p]],
                         out=xt[p][:, j, hf * 512:(hf + 1) * 512], in0=self.ps[b][:],
                         in1=xt[p][:, j, hf * 512:(hf + 1) * 512], op=ALU.add)
        self.dma("sync", drows, xt[p][:], [r_xt[p]], [self.r_xs])


def _phase_AB(self, l, x_src, x_dst):
    with contextlib.ExitStack() as st0:
        self.KT = self.sbt(st0, "KT", [128, 4, SEQ], BF16)
        self.krT = self.sbt(st0, "krT", [64, SEQ], BF16)
        self.V = self.sbt(st0, "V", [128, 32, 512], BF16)
        self.r_KT, self.r_krT, self.r_V = Res(), Res(), Res()
        if l == 0:
            z = self.sbt(st0, "zpad", [128, 4, 8], F32)
            rz = Res()
            self.vec("memset", [], [rz], ap=z[:], constant=0.0)
            self.dma("sync", self.u_d[:, 0:8].rearrange("(g p) t -> p g t", p=128), z[:], [rz], [self.r_ud])
            self.dma("sync", self.u_d[:, SEQ + 8:SEQ + 16].rearrange("(g p) t -> p g t", p=128), z[:], [rz],
                     [self.r_ud])
        with contextlib.ExitStack() as st:
            _phase_A(self, l, x_src, st)
        self.S.barrier()
        with contextlib.ExitStack() as st:
            _phase_B(self, l, x_src, x_dst, st)
        self.S.barrier()


MK.phase_AB = _phase_AB


def _phase_D(self, l, x_src):
    W = self.W
    nexp = self.nexp
    with contextlib.ExitStack() as st0:
        aff = self.sbt(st0, "aff", [128, 32, 16], F32)
        r_aff = Res()
        idx_all = self.sbt(st0, "idx_all", [128, 16, 4], I32)
        gate_all = self.sbt(st0, "gate_all", [128, 16, 4], F32)
        r_idx = Res()
        with contextlib.ExitStack() as st:
            gf, r_gf = self.load_gain_b(st, "gf", W["norm_ffn"][l:l + 1, :])
            wr = self.sbt(st, "wr32", [128, 8, 16], F32)
            r_wr = Res()
            self.dma("sync", wr[:], W["w_router"][l].rearrange("(k p) e -> p k e", p=128), [], [r_wr])
            xt = [self.sbt(st, "xtD", [128, 4, DM], F32) for _ in range(2)]
            r_xt = [Res(), Res()]
            h32 = self.sbt(st, "h32", [128, 4, DM], F32)
            hb = [self.sbt(st, "hbD", [128, 4, DM], BF16) for _ in range(2)]
            r_hb = [Res(), Res()]
            hT = self.sbt(st, "hT32", [128, 8, 512], F32)
            junk = self.sbt(st, "junkD", [128, DM], BF16)
            stat = self.sbt(st, "statD", [128, 24], F32)
            sm = self.sbt(st, "smD", [128, 16], F32)
            ex = self.sbt(st, "exD", [128, 4, 16], F32)
            r_h32, r_hT, r_junk, r_stat, r_sm, r_ex = (Res() for _ in range(6))
            for i in range(NT):
                p = i % 2
                rows = x_src[i * 512:(i + 1) * 512, :].rearrange("(j p) d -> p j d", p=128)
                self.dma("sync", xt[p][:], rows, [self.r_xs], [r_xt[p]])
                self.norm_tile(xt[p], r_xt[p], 4, gf, r_gf, h32, r_h32, junk, r_junk, stat, r_stat)
                for j in range(4):
                    self.act(hb[p][:, j, :], h32[:, j, :], AF.Copy, [r_h32], [r_hb[p]])
                self.dma("sync", self.htok_d[i * 512:(i + 1) * 512, :].rearrange("(j p) d -> p j d", p=128), hb[p][:],
                         [r_hb[p]], [self.r_htok])
                self.transpose_tile(h32, r_h32, 4, hT, r_hT, [0, 1], dt=F32, ident=self.ident_f)
                b = 2 + i % 2
                for j in range(4):
                    for k in range(8):
                        self.mm(self.ps[b][:, j * 16:(j + 1) * 16], hT[:, k, j * 128:(j + 1) * 128], wr[:, k, :],
                                k == 0, k == 7, [r_hT, r_wr], [self.psr[b]], inc=(k == 7))
                lg = self.ps[b][:, 0:64].rearrange("p (j e) -> p j e", e=16)
                self.vec("tensor_reduce", [self.psr[b]], [r_sm], out=sm[:, 0:4], in_=lg, axis=mybir.AxisListType.X,
                         op=ALU.max, negate=True)
                for j in range(4):
                    self.act(ex[:, j, :], self.ps[b][:, j * 16:(j + 1) * 16], AF.Exp, [self.psr[b], r_sm], [r_ex, r_sm],
                             bias=sm[:, j:j + 1], accum_out=sm[:, 4 + j:5 + j])
                self.vec("reciprocal", [r_sm], [r_sm], out=sm[:, 8:12], in_=sm[:, 4:8])
                for j in range(4):
                    self.vec("tensor_scalar", [r_ex, r_sm], [r_aff], out=aff[:, i * 4 + j, :], in0=ex[:, j, :],
                             scalar1=sm[:, 8 + j:9 + j], scalar2=None, op0=ALU.mult)
        self.S.barrier()
        with contextlib.ExitStack() as st:
            affE = self.sbt(st, "affE", [16, SEQ], F32)
            gselT = self.sbt(st, "gselT", [16, SEQ], F32)
            junkE = self.sbt(st, "junkE", [16, SEQ], BF16)
            bis = self.sbt(st, "bis", [16, 8], F32)
            r_affE, r_gselT, r_junkE, r_bis = Res(), Res(), Res(), Res()
            for c in range(32):
                b = (c // 4) % 2
                self.tr(self.ps[b][0:16, (c % 4) * 128:(c % 4 + 1) * 128], aff[:, c, :], self.ident_f[:],
                        [r_aff, self.r_const], [self.psr[b]], inc=(c % 4 == 3))
                if c % 4 == 3:
                    self.act(affE[:, (c - 3) * 128:(c + 1) * 128], self.ps[b][0:16, :], AF.Copy, [self.psr[b]], [r_affE])
            self.vec("memset", [], [r_bis], ap=bis[:], constant=0.0)
            for it in range(30):
                wdt = float(2.0 ** -(it + 1))
                self.vec("tensor_scalar", [r_bis], [r_bis], out=bis[:, 1:2], in0=bis[:, 0:1], scalar1=wdt, scalar2=None,
                         op0=ALU.add)
                self.vec("tensor_scalar", [r_affE, r_bis], [r_junkE, r_bis], out=junkE[:], in0=affE[:],
                         scalar1=bis[:, 1:2], scalar2=None, op0=ALU.is_gt, op1=ALU.add, accum_out=bis[:, 2:3])
                self.vec("tensor_scalar", [r_bis], [r_bis], out=bis[:, 3:4], in0=bis[:, 2:3], scalar1=float(CAP) - 0.5,
                         scalar2=wdt, op0=ALU.is_ge, op1=ALU.mult)
                self.vec("tensor_tensor", [r_bis], [r_bis], out=bis[:, 0:1], in0=bis[:, 0:1], in1=bis[:, 3:4], op=ALU.add)
            self.vec("scalar_tensor_tensor", [r_affE, r_bis], [r_gselT], out=gselT[:], in0=affE[:], scalar=bis[:, 0:1],
                     in1=affE[:], op0=ALU.is_gt, op1=ALU.mult)
            gsel = self.sbt(st, "gsel", [128, 512], F32)
            sel32 = self.sbt(st, "sel32", [128, 512], F32)
            selb = self.sbt(st, "selb", [128, 512], BF16)
            tril = self.sbt(st, "tril", [128, 128], BF16)
            pos = self.sbt(st, "pos", [128, 512], F32)
            pA = self.sbt(st, "pA", [128, 512], F32)
            pB = self.sbt(st, "pB", [128, 512], F32)
            Rr = self.sbt(st, "Rr", [128, 32, 16, 5], BF16)
            tok = self.sbt(st, "tok", [128, 32, 3], F32)
            iota = self.sbt(st, "iota", [128, 512], F32)
            ghi = self.sbt(st, "ghi", [128, 512], BF16)
            r_gsel, r_sel, r_tril, r_pos, r_pA, r_pB, r_Rr, r_tok, r_iota, r_ghi = (Res() for _ in range(10))
            self.dma("gpsimd", tril[:], self.C["c_tril"], [], [r_tril])
            self.dma("sync", tok[:], self.C["c_tok"].rearrange("p (c k) -> p c k", k=3), [], [r_tok])
            self.dma("sync", iota[:], self.C["c_iota"], [], [r_iota])
            b = 2
            for c in range(32):
                self.tr(self.ps[b][:, c * 16:(c + 1) * 16], gselT[0:16, c * 128:(c + 1) * 128], self.ident_f[0:16, 0:16],
                        [r_gselT, self.r_const], [self.psr[b]], inc=(c == 31))
            self.vec("tensor_copy", [self.psr[b]], [r_gsel], out=gsel[:], in_=self.ps[b][:])
            self.vec("tensor_single_scalar", [r_gsel], [r_sel], out=sel32[:], in_=gsel[:], scalar=0.0, op=ALU.is_gt)
            self.vec("tensor_copy", [r_sel], [r_sel], out=selb[:], in_=sel32[:])
            self.mm(self.ps[3][:], tril[:], selb[:], True, True, [r_tril, r_sel], [self.psr[3]])
            self.mm(self.ps[4][:], self.ones_b[:], selb[:], True, True, [self.r_const, r_sel], [self.psr[4]])
            self.vec("tensor_copy", [self.psr[4]], [r_pA], out=pA[:], in_=self.ps[4][:])
            cur, r_cur, oth, r_oth = pA, r_pA, pB, r_pB
            for s in (1, 2, 4, 8, 16):
                w = 16 * s
                self.vec("tensor_copy", [r_cur], [r_oth], out=oth[:, 0:w], in_=cur[:, 0:w])
                self.vec("tensor_tensor", [r_cur], [r_oth], out=oth[:, w:512], in0=cur[:, w:512], in1=cur[:, 0:512 - w],
                         op=ALU.add)
                cur, r_cur, oth, r_oth = oth, r_oth, cur, r_cur
            self.vec("tensor_tensor", [r_cur, self.psr[3]], [r_pos], out=pos[:], in0=self.ps[3][:], in1=cur[:], op=ALU.add)
            self.vec("tensor_tensor", [r_pos, self.psr[4]], [r_pos], out=pos[:], in0=pos[:], in1=self.ps[4][:],
                     op=ALU.subtract)
            for e in range(16):
                self.vec("tensor_copy", [r_tok], [r_Rr], out=Rr[:, :, e, 0:3], in_=tok[:])
            self.vec("tensor_copy", [r_gsel], [r_ghi], out=ghi[:], in_=gsel[:])
            self.vec("tensor_copy", [r_ghi], [r_Rr], out=Rr[:, :, :, 3], in_=ghi[:].rearrange("p (c e) -> p c e", e=16))
            self.vec("tensor_tensor", [r_gsel, r_ghi], [r_oth], out=oth[:], in0=gsel[:], in1=ghi[:], op=ALU.subtract)
            self.vec("tensor_copy", [r_oth], [r_Rr], out=Rr[:, :, :, 4], in_=oth[:].rearrange("p (c e) -> p c e", e=16))
            O = [self.sbt(st, "O", [128, 512], BF16) for _ in range(3)]
            r_O = [Res() for _ in range(3)]
            r5s = self.sbt(st, "r5s", [8, 512], F32)
            r5 = self.sbt(st, "r5", [128, 4, 8], F32)
            tt = self.sbt(st, "ttD", [128, 8], F32)
            r_r5s, r_r5, r_tt = Res(), Res(), Res()
            pos3 = pos[:].rearrange("p (c e) -> p c e", e=16)
            sel3 = sel32[:].rearrange("p (c e) -> p c e", e=16)
            oi = 0
            for e in range(nexp):
                b = 5 + e % 2
                for c in range(32):
                    o, r_o = O[oi % 3], r_O[oi % 3]
                    oi += 1
                    self.vec("tensor_scalar", [r_iota, r_pos, r_sel], [r_o], out=o[:], in0=iota[:],
                             scalar1=pos3[:, c, e:e + 1], scalar2=sel3[:, c, e:e + 1], op0=ALU.is_equal, op1=ALU.mult)
                    self.mm(self.ps[b][0:5, :], Rr[:, c, e, :], o[:], c == 0, c == 31, [r_Rr, r_o], [self.psr[b]],
                            inc=True)
                self.act(r5s[0:5, :], self.ps[b][0:5, :], AF.Copy, [self.psr[b]], [r_r5s])
                b2 = 7
                for j in range(4):
                    self.tr(self.ps[b2][:, j * 8:j * 8 + 5], r5s[0:5, j * 128:(j + 1) * 128], self.ident_f[0:5, 0:5],
                            [r_r5s, self.r_const], [self.psr[b2]], inc=(j == 3))
                self.vec("tensor_copy", [self.psr[b2]], [r_r5], out=r5[:],
                         in_=self.ps[b2][:, 0:32].rearrange("p (j k) -> p j k", k=8))
                self.vec("scalar_tensor_tensor", [r_r5], [r_tt], out=tt[:, 0:4], in0=r5[:, :, 0], scalar=64.0,
                         in1=r5[:, :, 1], op0=ALU.mult, op1=ALU.add)
                self.vec("tensor_scalar", [r_r5], [r_tt], out=tt[:, 4:8], in0=r5[:, :, 2], scalar1=-float(SEQ),
                         scalar2=float(SEQ), op0=ALU.mult, op1=ALU.add)
                self.vec("tensor_tensor", [r_tt], [r_tt], out=tt[:, 0:4], in0=tt[:, 0:4], in1=tt[:, 4:8], op=ALU.add)
                self.vec("tensor_copy", [r_tt], [r_idx], out=idx_all[:, e, :], in_=tt[:, 0:4])
                self.vec("tensor_tensor", [r_r5], [r_idx], out=gate_all[:, e, :], in0=r5[:, :, 3], in1=r5[:, :, 4],
                         op=ALU.add)
        self.S.barrier()
        with contextlib.ExitStack() as st:
            NS = 4
            wb = [self.sbt(st, "wbD", [128, 16, 512], BF16) for _ in range(NS)]
            r_wb = [Res() for _ in range(NS)]
            xtok = [self.sbt(st, "xtok", [128, 4, DM], BF16) for _ in range(2)]
            r_xtok = [Res(), Res()]
            xgT = self.sbt(st, "xgT", [128, 8, 512], BF16)
            hid = self.sbt(st, "hid", [128, 16, 512], BF16)
            sg = [self.sbt(st, "sg", [128, 512], F32) for _ in range(2)]
            r_sg = [Res(), Res()]
            ygs = [self.sbt(st, "yg", [128, 4, DM], F32) for _ in range(2)]
            r_ygs = [Res(), Res()]
            r_xgT, r_hid = Res(), Res()
            prev_sc = []
            for p in range(2):
                self.vec("memset", [], [r_xtok[p]], ap=xtok[p][:], constant=0.0)
            units = [(e, u) for e in range(nexp) for u in range(6)]

            def load_unit(n):
                if n >= len(units):
                    return
                e, u = units[n]
                s = n % NS
                if u < 4:
                    gsrc = W["w_gate"][l][e][:, u * 512:(u + 1) * 512].rearrange("(k p) n -> p k n", p=128)
                    usrc = W["w_up"][l][e][:, u * 512:(u + 1) * 512].rearrange("(k p) n -> p k n", p=128)
                    self.dma("gpsimd", wb[s][:, 0:8, :], gsrc, [], [r_wb[s]])
                    self.dma("gpsimd", wb[s][:, 8:16, :], usrc, [], [r_wb[s]])
                else:
                    hf = u - 4
                    dsrc = W["w_down"][l][e][:, hf * 512:(hf + 1) * 512].rearrange("(f p) n -> p f n", p=128)
                    self.dma("gpsimd", wb[s][:, 0:8, :], dsrc[:, 0:8, :], [], [r_wb[s]])
                    self.dma("gpsimd", wb[s][:, 8:16, :], dsrc[:, 8:16, :], [], [r_wb[s]])

            def gather(e):
                if e >= nexp:
                    return
                for j in range(4):
                    self.S.dma("gpsimd", (lambda eng, e=e, j=j: eng.indirect_dma_start(
                        out=xtok[e % 2][:, j, :], out_offset=None, in_=self.htok_d[:, :],
                        in_offset=bass.IndirectOffsetOnAxis(ap=idx_all[:, e, j:j + 1], axis=0))),
                        [self.r_htok, r_idx], [r_xtok[e % 2]])

            LOOK = 3
            gather(0)
            for n in range(LOOK):
                load_unit(n)
            n = 0
            for e in range(nexp):
                gather(e + 1)
                self.transpose_tile(xtok[e % 2], r_xtok[e % 2], 4, xgT, r_xgT, [0, 1])
                for fb in range(4):
                    load_unit(n + LOOK)
                    s = n % NS
                    n += 1
                    for fc in range(4):
                        f = fb * 4 + fc
                        bg, bu = 2 + (f % 2) * 2, 3 + (f % 2) * 2
                        for k in range(8):
                            self.mm(self.ps[bg][:], wb[s][:, k, fc * 128:(fc + 1) * 128], xgT[:, k, :], k == 0, k == 7,
                                    [r_wb[s], r_xgT], [self.psr[bg]], inc=(k == 7))
                        for k in range(8):
                            self.mm(self.ps[bu][:], wb[s][:, 8 + k, fc * 128:(fc + 1) * 128], xgT[:, k, :], k == 0,
                                    k == 7, [r_wb[s], r_xgT], [self.psr[bu]], inc=(k == 7))
                        self.act(sg[f % 2][:], self.ps[bg][:], AF.Silu, [self.psr[bg]], [r_sg[f % 2]])
                        self.vec("tensor_tensor", [self.psr[bu], r_sg[f % 2]], [r_hid], out=hid[:, f, :],
                                 in0=self.ps[bu][:], in1=sg[f % 2][:], op=ALU.mult)
                yg, r_yg = ygs[e % 2], r_ygs[e % 2]
                for hf in range(2):
                    load_unit(n + LOOK)
                    s = n % NS
                    n += 1
                    for j in range(4):
                        b = 6 + j % 2
                        for f in range(16):
                            self.mm(self.ps[b][:], hid[:, f, j * 128:(j + 1) * 128], wb[s][:, f, :], f == 0, f == 15,
                                    [r_hid, r_wb[s]], [self.psr[b]], inc=(f == 15))
                        self.act(yg[:, j, hf * 512:(hf + 1) * 512], self.ps[b][:], AF.Copy, [self.psr[b], r_idx], [r_yg],
                                 scale=gate_all[:, e, j:j + 1])
                cur_sc = []
                for j in range(4):
                    cur_sc.append(self.S.dma("gpsimd", (lambda eng, e=e, j=j, yg=yg: eng.indirect_dma_start(
                        out=self.xs[:, :], out_offset=bass.IndirectOffsetOnAxis(ap=idx_all[:, e, j:j + 1], axis=0),
                        in_=yg[:, j, :], in_offset=None, compute_op=ALU.add)),
                        [r_yg, r_idx], [], extra=prev_sc))
                prev_sc = cur_sc
        self.S.barrier()


MK.phase_D = _phase_D
```
